# Optimizing a Trainium2 kernel written in Bass

```python
import math
import jax, jax.numpy as jnp
from jax import lax
import numpy as np

D_MODEL = 4096
BATCH = 2
SEQ = 4096
DEPTH = 2

HEAD_DIM = 128
W_A = 3 * D_MODEL // 8
W_B = D_MODEL // 4
W_C = 3 * D_MODEL // 8
H_A = W_A // HEAD_DIM
G_B = W_B // HEAD_DIM
H_C = W_C // HEAD_DIM
DK_C = HEAD_DIM // 2
CHUNK = 128
Q_BLOCK = 128
CONV_W = 4
N_BUCKETS = 32
MAX_DIST = 128
N_BRANCH = 3
D_FF = ((8 * D_MODEL // 3 + 255) // 256) * 256
N_EXPERTS = 8
TOP_K = 2
D_FF_EXPERT = 7 * D_MODEL // 8
N_DENSE = (DEPTH + 1) // 2
N_MOE = DEPTH // 2
EPS = 1e-6
IN_SIZES = (W_A, W_A, W_A, W_A, H_A, H_A, 2 * W_B, W_C, W_C, W_C, N_BRANCH * D_MODEL)
N_IN = sum(IN_SIZES)

kernel_name = 'hybrid_mlstm_gmlp_diffattn_moe'


def rms_norm(x, g):
    xf = x.astype(jnp.float32)
    xf = xf * lax.rsqrt(jnp.mean(xf * xf, axis=-1, keepdims=True) + EPS)
    return (xf * g.astype(jnp.float32)).astype(x.dtype)


def layer_norm(x, g, b):
    xf = x.astype(jnp.float32)
    mu = jnp.mean(xf, axis=-1, keepdims=True)
    xc = xf - mu
    xf = xc * lax.rsqrt(jnp.mean(xc * xc, axis=-1, keepdims=True) + EPS)
    return (xf * g.astype(jnp.float32) + b.astype(jnp.float32)).astype(x.dtype)


def split_columns(z):
    out, start = [], 0
    for s in IN_SIZES:
        out.append(z[..., start:start + s])
        start += s
    return out


def to_heads(t, n_heads):
    B, S, _ = t.shape
    return jnp.transpose(t.reshape(B, S, n_heads, -1), (0, 2, 1, 3))


def causal_dwconv(x, w):
    S = x.shape[1]
    xp = jnp.pad(x, ((0, 0), (CONV_W - 1, 0), (0, 0)))
    y = xp[:, 0:S] * w[0]
    for j in range(1, CONV_W):
        y = y + xp[:, j:j + S] * w[j]
    return y


def mlstm_chunkwise(q, k, v, i_pre, f_pre):
    B, H, S, DH = q.shape
    nc = S // CHUNK

    def chunks(t):
        return t.reshape(B, H, nc, CHUNK, *t.shape[3:])

    q, k, v = chunks(q), chunks(k), chunks(v)
    i_c = chunks(i_pre)
    b = jnp.cumsum(jax.nn.log_sigmoid(chunks(f_pre)), axis=-1)
    g = b[..., -1]
    a = g[..., None] - b + i_c

    def step(carry, inp):
        c, n, m = carry
        g_c, a_c, k_c, v_c = inp
        m_new = jnp.maximum(g_c + m, jnp.max(a_c, axis=-1))
        w = jnp.exp(a_c - m_new[..., None])
        decay = jnp.exp(g_c + m - m_new)
        c_new = decay[..., None, None] * c + jnp.einsum('bhl,bhlk,bhlv->bhkv', w, k_c, v_c)
        n_new = decay[..., None] * n + jnp.einsum('bhl,bhlk->bhk', w, k_c)
        return (c_new, n_new, m_new), (c, n, m)

    xs = tuple(jnp.moveaxis(t, 2, 0) for t in (g, a, k, v))
    init = (jnp.zeros((B, H, DH, DH), jnp.float32),
            jnp.zeros((B, H, DH), jnp.float32),
            jnp.zeros((B, H), jnp.float32))
    _, (c_prev, n_prev, m_prev) = lax.scan(step, init, xs)
    c_prev = jnp.moveaxis(c_prev, 0, 2)
    n_prev = jnp.moveaxis(n_prev, 0, 2)
    m_prev = jnp.moveaxis(m_prev, 0, 2)

    causal = jnp.tril(jnp.ones((CHUNK, CHUNK), dtype=bool))
    log_d = jnp.where(causal, b[..., :, None] - b[..., None, :] + i_c[..., None, :], -jnp.inf)
    log_inter = b + m_prev[..., None]
    m_t = jnp.maximum(log_inter, jnp.max(log_d, axis=-1))
    w = jnp.exp(log_d - m_t[..., None]) * jnp.einsum('bhctd,bhcsd->bhcts', q, k)
    e_inter = jnp.exp(log_inter - m_t)
    num = (e_inter[..., None] * jnp.einsum('bhctk,bhckv->bhctv', q, c_prev)
           + jnp.einsum('bhcts,bhcsv->bhctv', w, v))
    den = e_inter * jnp.einsum('bhctk,bhck->bhct', q, n_prev) + jnp.sum(w, axis=-1)
    h = num / jnp.maximum(jnp.abs(den), jnp.exp(-m_t))[..., None]
    return h.reshape(B, H, S, DH)


def spatial_gating(bz, ln_g, ln_b, w_s, b_s):
    B, S, _ = bz.shape
    z = jax.nn.gelu(bz)
    u, v = z[..., :W_B], z[..., W_B:]
    v = layer_norm(v, ln_g, ln_b)
    nc = S // CHUNK
    v = v.reshape(B, nc, CHUNK, G_B, HEAD_DIM)
    causal = jnp.tril(jnp.ones((CHUNK, CHUNK), dtype=bool))
    w = jnp.where(causal, w_s, jnp.zeros_like(w_s))
    s = jnp.einsum('gts,bcsgd->bctgd', w, v) + jnp.transpose(b_s)[:, :, None]
    return u * s.reshape(B, S, W_B)


def t5_bucket(dist):
    n = jnp.maximum(dist, 0)
    max_exact = N_BUCKETS // 2
    nf = jnp.maximum(n, 1).astype(jnp.float32)
    large = max_exact + (jnp.log(nf / max_exact) / math.log(MAX_DIST / max_exact)
                         * (N_BUCKETS - max_exact)).astype(jnp.int32)
    large = jnp.minimum(large, N_BUCKETS - 1)
    return jnp.where(n < max_exact, n, large)


def diff_attention(q1, q2, k1, k2, v, lam, rel_bias):
    B, H, S, Dk = q1.shape
    nb = S // Q_BLOCK
    scale = Dk ** -0.5
    vf = v.astype(jnp.float32)
    kpos = jnp.arange(S, dtype=jnp.int32)

    def to_blocks(t):
        return jnp.moveaxis(t.reshape(B, H, nb, Q_BLOCK, t.shape[-1]), 2, 0)

    def one_block(args):
        q1b, q2b, start = args
        qpos = start + jnp.arange(Q_BLOCK, dtype=jnp.int32)
        dist = qpos[:, None] - kpos[None, :]
        bias = jnp.transpose(rel_bias[t5_bucket(dist)], (2, 0, 1)).astype(jnp.float32)
        causal = dist >= 0

        def probs(qb, kk):
            s = jnp.einsum('bhqd,bhkd->bhqk', qb, kk).astype(jnp.float32) * scale + bias
            return jax.nn.softmax(jnp.where(causal, s, -jnp.inf), axis=-1)

        a = probs(q1b, k1) - lam * probs(q2b, k2)
        return jnp.einsum('bhqk,bhkd->bhqd', a, vf)

    starts = jnp.arange(nb, dtype=jnp.int32) * Q_BLOCK
    out = lax.map(one_block, (to_blocks(q1), to_blocks(q2), starts))
    return jnp.moveaxis(out, 0, 2).reshape(B, H, S, -1)


def hybrid_mixer(h, layer_idx, w_in, a_gate_bias, a_conv, a_norm, b_ln_g, b_ln_b, b_ws, b_bs,
                 c_lambda, c_norm, rel_bias, w_br_a, w_br_b, w_br_c, w_out):
    B, S, _ = h.shape
    f32 = jnp.float32
    z = h @ w_in
    aq, ak, av, ao, ai, af, bz, cq, ck, cv, gpre = split_columns(z)

    qk = jax.nn.silu(causal_dwconv(jnp.concatenate([aq, ak], axis=-1), a_conv))
    q = to_heads(qk[..., :W_A], H_A).astype(f32)
    k = to_heads(qk[..., W_A:], H_A).astype(f32) * (HEAD_DIM ** -0.5)
    v = to_heads(av, H_A).astype(f32)
    gb = a_gate_bias.astype(f32)
    i_pre = jnp.transpose(ai.astype(f32) + gb[:H_A], (0, 2, 1))
    f_pre = jnp.transpose(af.astype(f32) + gb[H_A:], (0, 2, 1))
    h_cell = jnp.transpose(mlstm_chunkwise(q, k, v, i_pre, f_pre), (0, 2, 1, 3))
    h_cell = jax.nn.sigmoid(ao.astype(f32)).reshape(B, S, H_A, HEAD_DIM) * h_cell
    y_a = rms_norm(h_cell, a_norm.reshape(H_A, HEAD_DIM)).reshape(B, S, W_A).astype(h.dtype)

    y_b = spatial_gating(bz, b_ln_g, b_ln_b, b_ws, b_bs)

    qc, kc, vc = to_heads(cq, H_C), to_heads(ck, H_C), to_heads(cv, H_C)
    lam_init = 0.8 - 0.6 * math.exp(-0.3 * layer_idx)
    lf = c_lambda.astype(f32)
    lam = jnp.exp(jnp.sum(lf[0] * lf[1])) - jnp.exp(jnp.sum(lf[2] * lf[3])) + lam_init
    o = diff_attention(qc[..., :DK_C], qc[..., DK_C:], kc[..., :DK_C], kc[..., DK_C:], vc, lam, rel_bias)
    o = rms_norm(o, c_norm) * (1.0 - lam_init)
    y_c = jnp.transpose(o, (0, 2, 1, 3)).reshape(B, S, W_C).astype(h.dtype)

    gates = jax.nn.sigmoid(gpre).reshape(B, S, N_BRANCH, D_MODEL)
    merged = (gates[:, :, 0] * (y_a @ w_br_a)
              + gates[:, :, 1] * (y_b @ w_br_b)
              + gates[:, :, 2] * (y_c @ w_br_c))
    return merged @ w_out


def swiglu(t, wg, wu, wd):
    return (jax.nn.silu(t @ wg) * (t @ wu)) @ wd


def moe_swiglu(h, router, wg, wu, wd):
    B, S, D = h.shape
    t = h.reshape(-1, D)
    logits = (t @ router).astype(jnp.float32)
    top_vals, top_idx = lax.top_k(logits, TOP_K)
    top_w = jax.nn.softmax(top_vals, axis=-1)
    combine = jnp.sum(jax.nn.one_hot(top_idx, N_EXPERTS, dtype=jnp.float32) * top_w[..., None], axis=1)
    out = jnp.zeros(t.shape, jnp.float32)
    for e in range(N_EXPERTS):
        out = out + combine[:, e:e + 1] * swiglu(t, wg[e], wu[e], wd[e]).astype(jnp.float32)
    return out.astype(h.dtype).reshape(B, S, D)


def setup_inputs(seed: int = 0) -> dict:
    key = jax.random.key(seed)
    ks = jax.random.split(key, 32)
    f32 = jnp.float32

    def nrm(k, shape, scale):
        return jax.random.normal(k, shape, f32) * scale

    def gain(k, shape):
        return 1.0 + nrm(k, shape, 0.02)

    i_bias = nrm(ks[3], (DEPTH, H_A), 0.1)
    f_bias = jnp.linspace(3.0, 6.0, H_A, dtype=f32)[None, :] + nrm(ks[4], (DEPTH, H_A), 0.02)
    return {
        'x': nrm(ks[0], (BATCH, SEQ, D_MODEL), 1.0),
        'norm_mix': gain(ks[1], (DEPTH, D_MODEL)),
        'w_in': nrm(ks[2], (DEPTH, D_MODEL, N_IN), D_MODEL ** -0.5),
        'a_gate_bias': jnp.concatenate([i_bias, f_bias], axis=-1),
        'a_conv': nrm(ks[5], (DEPTH, CONV_W, 2 * W_A), CONV_W ** -0.5),
        'a_norm': gain(ks[6], (DEPTH, W_A)),
        'b_ln_g': gain(ks[7], (DEPTH, W_B)),
        'b_ln_b': nrm(ks[8], (DEPTH, W_B), 0.02),
        'b_ws': nrm(ks[9], (DEPTH, G_B, CHUNK, CHUNK), CHUNK ** -0.5),
        'b_bs': gain(ks[10], (DEPTH, G_B, CHUNK)),
        'c_lambda': nrm(ks[11], (DEPTH, 4, DK_C), 0.1),
        'c_norm': gain(ks[12], (DEPTH, HEAD_DIM)),
        'rel_bias': nrm(ks[13], (N_BUCKETS, H_C), 0.5),
        'w_br_a': nrm(ks[14], (DEPTH, W_A, D_MODEL), W_A ** -0.5),
        'w_br_b': nrm(ks[15], (DEPTH, W_B, D_MODEL), W_B ** -0.5),
        'w_br_c': nrm(ks[16], (DEPTH, W_C, D_MODEL), W_C ** -0.5),
        'w_out': nrm(ks[17], (DEPTH, D_MODEL, D_MODEL), D_MODEL ** -0.5),
        'norm_ffn': gain(ks[18], (DEPTH, D_MODEL)),
        'ffn_wg': nrm(ks[19], (N_DENSE, D_MODEL, D_FF), D_MODEL ** -0.5),
        'ffn_wu': nrm(ks[20], (N_DENSE, D_MODEL, D_FF), D_MODEL ** -0.5),
        'ffn_wd': nrm(ks[21], (N_DENSE, D_FF, D_MODEL), D_FF ** -0.5),
        'router': nrm(ks[22], (N_MOE, D_MODEL, N_EXPERTS), D_MODEL ** -0.5),
        'moe_wg': nrm(ks[23], (N_MOE, N_EXPERTS, D_MODEL, D_FF_EXPERT), D_MODEL ** -0.5),
        'moe_wu': nrm(ks[24], (N_MOE, N_EXPERTS, D_MODEL, D_FF_EXPERT), D_MODEL ** -0.5),
        'moe_wd': nrm(ks[25], (N_MOE, N_EXPERTS, D_FF_EXPERT, D_MODEL), D_FF_EXPERT ** -0.5),
        'final_norm': gain(ks[26], (D_MODEL,)),
    }


def reference(x, norm_mix, w_in, a_gate_bias, a_conv, a_norm, b_ln_g, b_ln_b, b_ws, b_bs,
              c_lambda, c_norm, rel_bias, w_br_a, w_br_b, w_br_c, w_out, norm_ffn,
              ffn_wg, ffn_wu, ffn_wd, router, moe_wg, moe_wu, moe_wd, final_norm):
    h = x
    for li in range(DEPTH):
        h = h + hybrid_mixer(rms_norm(h, norm_mix[li]), li, w_in[li], a_gate_bias[li], a_conv[li],
                             a_norm[li], b_ln_g[li], b_ln_b[li], b_ws[li], b_bs[li], c_lambda[li],
                             c_norm[li], rel_bias, w_br_a[li], w_br_b[li], w_br_c[li], w_out[li])
        hn = rms_norm(h, norm_ffn[li])
        if li % 2 == 0:
            j = li // 2
            h = h + swiglu(hn, ffn_wg[j], ffn_wu[j], ffn_wd[j])
        else:
            j = li // 2
            h = h + moe_swiglu(hn, router[j], moe_wg[j], moe_wu[j], moe_wd[j])
    return rms_norm(h, final_norm)
```

```python
import math
import numpy as np
import concourse.bass as bass
import concourse.mybir as mybir
from concourse.bass_utils import run_bass_kernel_spmd

F32 = mybir.dt.float32
BF16 = mybir.dt.bfloat16
AF = mybir.ActivationFunctionType
ALU = mybir.AluOpType
AX = mybir.AxisListType
ENGS = ("pe", "act", "dve", "pool", "sp")
EPS = 1e-6
NEG_BIG = -30000.0


class Buf:
    __slots__ = ("name", "last_w", "readers", "dma_sem", "dma_cnt", "pw")

    def __init__(self, name=""):
        self.name = name
        self.last_w = None
        self.readers = []
        self.dma_sem = None
        self.dma_cnt = 0
        self.pw = []


class Ins:
    __slots__ = ("eng", "fn", "deps", "is_dma", "dma_sem", "dma_val", "needed", "sigval")

    def __init__(self, eng, fn):
        self.eng = eng
        self.fn = fn
        self.deps = []
        self.is_dma = False
        self.dma_sem = None
        self.dma_val = 0
        self.needed = False
        self.sigval = 0


class Prog:
    def __init__(self, nc):
        self.nc = nc
        self.streams = {e: [] for e in ENGS}
        self._ctx = []
        self.n_dma_sems = 0
        self.final_waits = []
        self.epoch = None
        self.all_bufs = []
        self.sem_pool = []

    def enter(self, cm):
        v = cm.__enter__()
        self._ctx.append(cm)
        return v

    def buf(self, name=""):
        b = Buf(name)
        self.all_bufs.append(b)
        return b

    def sbuf(self, name, shape, dt):
        return self.enter(self.nc.sbuf_tensor(name, list(shape), dt))

    def psum(self, name, shape, dt=F32):
        return self.enter(self.nc.psum_tensor(name, list(shape), dt))

    def close(self):
        while self._ctx:
            self._ctx.pop().__exit__(None, None, None)

    def _track(self, ins, reads, writes, appends=()):
        if self.epoch is not None:
            ins.deps.append(self.epoch)
        for b in reads:
            if b.last_w is not None:
                ins.deps.append(b.last_w)
            ins.deps.extend(b.pw)
            b.readers.append(ins)
        for b in writes:
            if b.last_w is not None:
                ins.deps.append(b.last_w)
            ins.deps.extend(b.pw)
            for r in b.readers:
                if r is not ins:
                    ins.deps.append(r)
            b.readers = []
            b.pw = []
            b.last_w = ins
        for b in appends:
            if b.last_w is not None:
                ins.deps.append(b.last_w)
            for r in b.readers:
                if r is not ins:
                    ins.deps.append(r)
            b.readers = []
            b.pw.append(ins)

    def op(self, eng, fn, reads=(), writes=()):
        ins = Ins(eng, fn)
        self._track(ins, reads, writes)
        self.streams[eng].append(ins)
        return ins

    def dma(self, eng, out, in_, reads=(), writes=(), sem_buf=None, appends=(), **kw):
        sb = sem_buf if sem_buf is not None else writes[0]
        if sb.dma_sem is None:
            if self.sem_pool:
                sb.dma_sem = self.sem_pool.pop()
            else:
                sb.dma_sem = [self.enter(self.nc.semaphore("dsem%d" % self.n_dma_sems)), 0]
                self.n_dma_sems += 1
        sb.dma_sem[1] += 16
        ins = Ins(eng, lambda e: e.dma_start(out=out, in_=in_, **kw))
        ins.is_dma = True
        ins.dma_sem = sb.dma_sem[0]
        ins.dma_val = sb.dma_sem[1]
        self._track(ins, reads, writes, appends)
        self.streams[eng].append(ins)
        return ins

    def barrier(self):
        nc = self.nc
        ins = Ins("dve", lambda e: e.engine_nop())
        if self.epoch is not None:
            ins.deps.append(self.epoch)
        for b in self.all_bufs:
            if b.last_w is not None:
                ins.deps.append(b.last_w)
            ins.deps.extend(b.readers)
            ins.deps.extend(b.pw)
            b.readers = []
            b.pw = []
            b.last_w = None
            if b.dma_sem is not None:
                self.sem_pool.append(b.dma_sem)
                b.dma_sem = None
        self.streams["dve"].append(ins)
        self.epoch = ins
        return ins

    def wait_all_at_end(self, eng, inss):
        self.final_waits.append((eng, list(inss)))

    def emit(self):
        nc = self.nc
        for eng, inss in self.final_waits:
            ins = Ins(eng, None)
            ins.deps = list(inss)
            self.streams[eng].append(ins)

        def skip(d, ins):
            return (not d.is_dma) and d.eng == ins.eng and d.eng in ("pe", "sp") and not ins.is_dma

        for e in ENGS:
            for ins in self.streams[e]:
                for d in ins.deps:
                    if d.is_dma or skip(d, ins):
                        continue
                    d.needed = True
        for e in ENGS:
            c = 0
            for ins in self.streams[e]:
                if ins.needed and not ins.is_dma:
                    c += 1
                    ins.sigval = c
        esem = {e: self.enter(nc.semaphore("esem_" + e)) for e in ENGS}
        block = self.enter(nc.Block())
        prog = self

        def run_stream(ename, eh):
            waited = {}
            for ins in prog.streams[ename]:
                req = {}
                for d in ins.deps:
                    if d.is_dma:
                        key, sem, val = id(d.dma_sem), d.dma_sem, d.dma_val
                    else:
                        if skip(d, ins):
                            continue
                        key, sem, val = d.eng, esem[d.eng], d.sigval
                    if waited.get(key, 0) >= val:
                        continue
                    if key not in req or req[key][1] < val:
                        req[key] = (sem, val)
                for key, (sem, val) in req.items():
                    eh.wait_ge(sem, val)
                    waited[key] = val
                if ins.fn is None:
                    continue
                bi = ins.fn(eh)
                if ins.is_dma:
                    bi.then_inc(ins.dma_sem, 16)
                elif ins.needed:
                    bi.then_inc(esem[ename], 1)

        @block.tensor
        def _(eh):
            run_stream("pe", eh)

        @block.scalar
        def _(eh):
            run_stream("act", eh)

        @block.vector
        def _(eh):
            run_stream("dve", eh)

        @block.gpsimd
        def _(eh):
            run_stream("pool", eh)

        @block.sync
        def _(eh):
            run_stream("sp", eh)

        self.close()


class Cfg:
    def __init__(self, D=4096, T=4096, NE=8):
        self.D, self.T, self.NE = D, T, NE
        self.L = 2
        self.WA = 3 * D // 8
        self.WB = D // 4
        self.WC = 3 * D // 8
        self.HA, self.GB, self.HC = self.WA // 128, self.WB // 128, self.WC // 128
        self.DFF = ((8 * D // 3 + 255) // 256) * 256
        self.DFFE = 7 * D // 8
        self.KC = D // 128
        self.NCH = T // 128
        self.sizes = (self.WA, self.WA, self.WA, self.WA, self.HA, self.HA, 2 * self.WB,
                      self.WC, self.WC, self.WC, 3 * D)
        self.NIN = sum(self.sizes)
        self.TTA = min(T, 1024)
        self.TT = 512
        o = [0]
        for s in self.sizes:
            o.append(o[-1] + s)
        self.off = o


C_ID, C_J, C_TRIU, C_BIG, C_OH, C_SEL, C_END = 0, 128, 256, 384, 896, 1280, 1280 + 12 * 128


def t5_bucket_np(n):
    n = np.maximum(n, 0)
    nf = np.maximum(n, 1).astype(np.float32)
    large = 16 + (np.log(nf / 16) / math.log(128 / 16) * 16).astype(np.int32)
    large = np.minimum(large, 31)
    return np.where(n < 16, n, large)


def make_consts():
    c = np.zeros((128, C_END), np.float32)
    c[:, C_ID:C_ID + 128] = np.eye(128)
    c[:, C_J:C_J + 128] = np.eye(128)[::-1]
    s = np.arange(128)[:, None]
    t = np.arange(128)[None, :]
    c[:, C_TRIU:C_TRIU + 128] = (s <= t)
    c[:, C_BIG:C_BIG + 512] = np.tile(np.where(s > t, 1e4, 0.0), (1, 4))
    j = np.arange(384)
    dist = j - 127
    b = t5_bucket_np(dist)
    for jj in range(384):
        if dist[jj] >= 0:
            c[b[jj], C_OH + jj] = 1.0
        else:
            c[32, C_OH + jj] = NEG_BIG
    for h in range(12):
        c[h, C_SEL + h * 128:C_SEL + (h + 1) * 128] = 1.0
    return c


class Builder:
    def __init__(self, cfg, debug=False):
        self.cfg = cfg
        self.debug = debug
        nc = bass.Bass("TRN2", target_bir_lowering=False)
        self.nc = nc
        self.P = Prog(nc)
        c = cfg
        L = c.L

        def inp(name, shape):
            return nc.dram_tensor(name, list(shape), F32, kind="ExternalInput").ap()

        self.xT = inp("xT", [c.D, c.T])
        self.norm_mix = inp("norm_mix", [L, 128, c.KC])
        self.w_in = inp("w_in", [L, c.D, c.NIN])
        self.a_gate_bias = inp("a_gate_bias", [L, 2 * c.HA, 1])
        self.a_conv = inp("a_conv", [L, 2 * c.WA, 4])
        self.a_norm = inp("a_norm", [L, c.WA])
        self.b_ln_g = inp("b_ln_g", [L, c.WB])
        self.b_ln_b = inp("b_ln_b", [L, c.WB])
        self.b_wsT = inp("b_wsT", [L, c.GB, 128, 128])
        self.b_bs = inp("b_bs", [L, c.GB * 128])
        self.c_lambda = inp("c_lambda", [L, 256])
        self.c_norm = inp("c_norm", [L, 128])
        self.rel_bias = inp("rel_bias", [32, c.HC])
        self.w_br_a = inp("w_br_a", [L, c.WA, c.D])
        self.w_br_b = inp("w_br_b", [L, c.WB, c.D])
        self.w_br_c = inp("w_br_c", [L, c.WC, c.D])
        self.w_out = inp("w_out", [L, c.D, c.D])
        self.norm_ffn = inp("norm_ffn", [L, 128, c.KC])
        self.ffn_wg = inp("ffn_wg", [1, c.D, c.DFF])
        self.ffn_wu = inp("ffn_wu", [1, c.D, c.DFF])
        self.ffn_wd = inp("ffn_wd", [1, c.DFF, c.D])
        self.router = inp("router", [1, c.D, c.NE])
        self.moe_wg = inp("moe_wg", [1, c.NE, c.D, c.DFFE])
        self.moe_wu = inp("moe_wu", [1, c.NE, c.D, c.DFFE])
        self.moe_wd = inp("moe_wd", [1, c.NE, c.DFFE, c.D])
        self.final_norm = inp("final_norm", [128, c.KC])
        self.consts = inp("consts", [128, C_END])
        self.outT = nc.dram_tensor("outT", [c.D, c.T], F32, kind="ExternalOutput").ap()

        def scr(name, shape, dt):
            kind = "ExternalOutput" if (debug and name in debug) else "Internal"
            return nc.dram_tensor(name, list(shape), dt, kind=kind).ap()

        self.hbuf = scr("hbuf", [c.D, c.T], F32)
        self.aqkT = scr("aqkT", [2 * c.WA, c.T], F32)
        self.av = scr("av", [c.T, c.WA], BF16)
        self.ao = scr("ao", [c.T, c.WA], F32)
        self.aifT = scr("aifT", [2 * c.HA, c.T], F32)
        self.bzuT = scr("bzuT", [c.WB, c.T], F32)
        self.bzv = scr("bzv", [c.T, c.WB], F32)
        self.cqkT = scr("cqkT", [2 * c.WC, c.T], BF16)
        self.cv = scr("cv", [c.T, c.WC], BF16)
        self.gates = scr("gates", [3 * c.D, c.T], F32)
        self.yT = scr("yT", [c.D, c.T], BF16)
        self.text = scr("text", [c.HC, 384], F32)
        self.dram_bufs = {}

        P = self.P
        self.ARENA = 105000
        self.arena = P.sbuf("arena", [128, self.ARENA], BF16)
        self.ps = [P.psum("psb%d" % i, [128, 512])[:] for i in range(8)]
        self.psb = [P.buf("ps%d" % i) for i in range(8)]
        self.ps_rr = 0
        self.top = 0
        self.persist_top = 0
        self.out_stores = []

    def tile(self, shape, dt, name="t"):
        n = int(np.prod(shape[1:]))
        sz = 4 if dt == F32 else 2
        nb = (n * sz + 63) // 64 * 64
        off = self.top
        self.top += nb
        assert self.top <= self.ARENA * 2, ("arena overflow", name, self.top)
        a = self.arena[0:shape[0], off // 2: off // 2 + n * sz // 2]
        if dt == F32:
            a = a.bitcast(F32)
        if len(shape) == 3:
            a = a.rearrange("p (a b) -> p a b", b=shape[2])
        elif len(shape) == 4:
            a = a.rearrange("p (a b c) -> p a b c", b=shape[2], c=shape[3])
        return a, self.P.buf(name)

    def phase(self):
        self.P.barrier()
        self.top = self.persist_top
        self.psb = [self.P.buf("ps%d" % i) for i in range(8)]

    def dbuf(self, name):
        if name not in self.dram_bufs:
            self.dram_bufs[name] = Buf(name)
        b = self.dram_bufs[name]
        if b not in self.P.all_bufs:
            self.P.all_bufs.append(b)
        return b

    def mm(self, out, lhsT, rhs, start, stop, reads, writes):
        return self.P.op("pe", lambda e: e.matmul(out, lhsT, rhs, start=start, stop=stop), reads, writes)

    def act(self, out, in_, func, reads, writes, bias=None, scale=1.0, accum_out=None):
        kw = {}
        if bias is not None:
            kw["bias"] = bias
        if accum_out is not None:
            kw["accum_out"] = accum_out
        return self.P.op("act", lambda e: e.activation(out, in_, func, scale=scale, **kw), reads, writes)

    def tt(self, out, a, b, op, reads, writes, eng="dve"):
        return self.P.op(eng, lambda e: e.tensor_tensor(out, a, b, op), reads, writes)

    def ts(self, out, in0, s1, s2, op0, op1, reads, writes, eng="dve"):
        if op1 is None:
            return self.P.op(eng, lambda e: e.tensor_scalar(out, in0, s1, None, op0), reads, writes)
        return self.P.op(eng, lambda e: e.tensor_scalar(out, in0, s1, s2, op0, op1), reads, writes)

    def stt(self, out, in0, scalar, in1, op0, op1, reads, writes, eng="dve"):
        return self.P.op(eng, lambda e: e.scalar_tensor_tensor(out, in0, scalar, in1, op0, op1), reads, writes)

    def cp(self, out, in_, reads, writes, eng="dve"):
        if eng == "act":
            return self.P.op("act", lambda e: e.copy(out, in_), reads, writes)
        return self.P.op(eng, lambda e: e.tensor_copy(out, in_), reads, writes)

    def memset(self, out, val, writes, eng="dve"):
        return self.P.op(eng, lambda e: e.memset(out, val), (), writes)

    def recip(self, out, in_, reads, writes):
        return self.P.op("dve", lambda e: e.reciprocal(out, in_), reads, writes)

    def scan(self, out, d0, d1, init, op0, op1, reads, writes):
        return self.P.op("dve", lambda e: e.tensor_tensor_scan(out, d0, d1, init, op0, op1), reads, writes)

    def reduce(self, out, in_, op, reads, writes):
        return self.P.op("dve", lambda e: e.tensor_reduce(out, in_, AX.X, op), reads, writes)

    def ld(self, out, in_, writes, reads=(), eng="sp", **kw):
        return self.P.dma(eng, out, in_, reads=reads, writes=writes, **kw)

    def st(self, out, in_, reads, dname, eng="sp"):
        b = self.dbuf(dname)
        return self.P.dma(eng, out, in_, reads=list(reads), writes=[], sem_buf=reads[0], appends=[b])

    def nps(self, lo=0, hi=6):
        i = lo + (self.ps_rr % (hi - lo))
        self.ps_rr += 1
        return self.ps[i], self.psb[i]

    def setup(self):
        c = self.cfg
        self.cst, self.bcst = self.tile([128, C_END], F32, "consts")
        self.ld(self.cst, self.consts, [self.bcst])
        self.idb, self.bidb = self.tile([128, 128], BF16, "idb")
        self.cp(self.idb, self.cst[:, C_ID:C_ID + 128], [self.bcst], [self.bidb])
        self.onesb, self.bonesb = self.tile([128, 128], BF16, "onesb")
        self.memset(self.onesb, 1.0, [self.bonesb])
        self.zb, self.bzb = self.tile([1, 512], BF16, "zb")
        self.memset(self.zb, 0.0, [self.bzb])
        self.cc, self.bcc = self.tile([128, 8], F32, "constcols")
        self.memset(self.cc[:, 0:1], EPS, [self.bcc])
        self.memset(self.cc[:, 1:2], 1.0, [self.bcc])
        self.memset(self.cc[:, 2:3], 0.0, [self.bcc])
        self.NSLOT = 3
        self.SLOTE = 12288
        self.BD, self.bBD = self.tile([128, c.HC, 128], F32, "BD")
        self.BO, self.bBO = self.tile([128, c.HC, 128], F32, "BO")
        self.c31, self.bc31 = self.tile([128, c.HC], F32, "c31")
        self.persist_top = self.top
        self.build_attn_bias()

    def alloc_slots(self, staging):
        self.slots = [self.tile([128, self.SLOTE], BF16, "slot%d" % i) for i in range(self.NSLOT)]
        self.slot_rr = 0
        if staging:
            self.stg = [self.tile([128, 512], F32, "stg%d" % i) for i in range(4)]
            self.stg_rr = 0

    def nstg(self):
        t = self.stg[self.stg_rr % len(self.stg)]
        self.stg_rr += 1
        return t

    def wload(self, pieces, cols):
        slot, sb = self.slots[self.slot_rr % self.NSLOT]
        self.slot_rr += 1
        ktot = max(k0 + ap.shape[0] // 128 for ap, k0 in pieces)
        assert ktot * cols <= self.SLOTE, (ktot, cols)
        view = slot[:, 0:ktot * cols].rearrange("p (k f) -> p k f", f=cols)
        for ap, k0 in pieces:
            kc = ap.shape[0] // 128
            self.P.dma("pool", view[:, k0:k0 + kc, :], ap.rearrange("(k p) f -> p k f", p=128), writes=[sb])
        return view, sb

    def build_attn_bias(self):
        c = self.cfg
        self.top = self.persist_top
        rb, brb = self.tile([33, c.HC], F32, "rb")
        self.memset(rb, 1.0, [brb])
        self.ld(rb[0:32, :], self.rel_bias, [brb])
        ps, pb = self.nps()
        self.mm(ps[0:c.HC, 0:384], rb, self.cst[0:33, C_OH:C_OH + 384], True, True, [brb, self.bcst], [pb])
        tx, btx = self.tile([c.HC, 384], F32, "tx")
        self.cp(tx, ps[0:c.HC, 0:384], [pb], [btx])
        s = self.st(self.text, tx, [btx], "text")
        hk, bhk = self.tile([128, 2, c.HC, 128], F32, "hk")
        for h in range(c.HC):
            for o, off in enumerate((0, 128)):
                src = bass.AP(tensor=self.text.tensor, offset=h * 384 + off, ap=[[1, 128], [1, 128]])
                self.P.dma("sp", hk[:, o, h, :], src, reads=[self.dbuf("text")], writes=[bhk])
        for h in range(c.HC):
            for o, (dst, bdst) in enumerate(((self.BD, self.bBD), (self.BO, self.bBO))):
                ps, pb = self.nps()
                self.mm(ps[:, 0:128], self.cst[:, C_J:C_J + 128], hk[:, o, h, :], True, True, [self.bcst, bhk], [pb])
                self.cp(dst[:, h, :], ps[:, 0:128], [pb], [bdst])
        self.ld(self.c31, self.rel_bias[31].partition_broadcast(128), [self.bc31])
        self.top = self.persist_top

    def norm_alloc(self, TT):
        xb = [self.tile([128, TT], F32, "nx%d" % i) for i in range(getattr(self, "_nxb", 3))]
        sq = [self.tile([128, TT], BF16, "nsq%d" % i) for i in range(2)]
        rs = self.tile([128, TT], F32, "rstd")
        return (xb, sq, rs)

    def norm_tile(self, scr, src, srcname, t0, TT, gamma, bgamma, out_bf=None, bout=None, out_dram=None,
                  router=None):
        c = self.cfg
        KC = c.KC
        nh = TT // 512
        xb, sq, (rstd, brstd) = scr
        pss = [(self.ps[6], self.psb[6]), (self.ps[7], self.psb[7])][:nh]
        srcb = self.dbuf(srcname)
        for k in range(KC):
            x, bx = xb[k % len(xb)]
            self.ld(x, src[k * 128:(k + 1) * 128, t0:t0 + TT], [bx], reads=[srcb])
            s, bs = sq[k % 2]
            self.act(s, x, AF.Square, [bx], [bs])
            for hh in range(nh):
                self.mm(pss[hh][0], self.onesb, s[:, hh * 512:(hh + 1) * 512], k == 0, k == KC - 1,
                        [bs, self.bonesb], [pss[hh][1]])
        for hh in range(nh):
            self.act(rstd[:, hh * 512:(hh + 1) * 512], pss[hh][0], AF.Sqrt, [pss[hh][1], self.bcc], [brstd],
                     bias=self.cc[:, 0:1], scale=1.0 / c.D)
        self.recip(rstd, rstd, [brstd], [brstd])
        if router is not None:
            rsb, brs, psl, pbl = router
            nl = (TT // 128) * c.NE
            self.mm(psl[:, 0:nl], self.zb[0:1, 0:128], self.zb[0:1, 0:nl], True, False, [self.bzb], [pbl])
        for k in range(KC):
            x, bx = xb[k % len(xb)]
            self.ld(x, src[k * 128:(k + 1) * 128, t0:t0 + TT], [bx], reads=[srcb])
            if out_bf is not None:
                self.stt(out_bf[:, k, :], x, gamma[:, k:k + 1], rstd, ALU.mult, ALU.mult, [bx, bgamma, brstd], [bout])
            if out_dram is not None or router is not None:
                self.stt(x, x, gamma[:, k:k + 1], rstd, ALU.mult, ALU.mult, [bx, bgamma, brstd], [bx])
            if out_dram is not None:
                self.out_stores.append(self.st(out_dram[k * 128:(k + 1) * 128, t0:t0 + TT], x, [bx], "outT"))
            if router is not None:
                for tb in range(TT // 128):
                    self.mm(psl[:, tb * c.NE:(tb + 1) * c.NE], x[:, tb * 128:(tb + 1) * 128], rsb[:, k, :],
                            False, k == KC - 1, [bx, brs], [pbl])

    def lin_fm(self, pieces_fn, ncols, rhs_fn, rhs_bufs, K, TT, evac, blk=None):
        if blk is None:
            blk = (self.SLOTE // K) // 128 * 128
            blk = min(blk, 384)
        c0 = 0
        while c0 < ncols:
            cw = min(blk, ncols - c0)
            view, sb = self.wload(pieces_fn(c0, cw), cw)
            for j0 in range(0, cw, 128):
                mc = min(128, cw - j0)
                for hh in range(TT // 512):
                    ps, pb = self.nps()
                    for k in range(K):
                        self.mm(ps[0:mc, :], view[:, k, j0:j0 + mc], rhs_fn(k, hh), k == 0, k == K - 1,
                                [sb] + rhs_bufs, [pb])
                    evac(c0 + j0, mc, hh, ps, pb)
            c0 += cw

    def lin_tm(self, pieces_fn, ncols, lhs_fn, lhs_bufs, K, TT, evac):
        blk = min((self.SLOTE // K) // 128 * 128, 384)
        c0 = 0
        while c0 < ncols:
            cw = min(blk, ncols - c0)
            view, sb = self.wload(pieces_fn(c0, cw), cw)
            for tb in range(TT // 128):
                ps, pb = self.nps()
                for k in range(K):
                    self.mm(ps[:, 0:cw], lhs_fn(k, tb), view[:, k, 0:cw], k == 0, k == K - 1, [sb] + lhs_bufs, [pb])
                evac(c0, cw, tb, ps, pb)
            c0 += cw

    def phase_a(self, li):
        c = self.cfg
        self.phase()
        src, srcname = (self.xT, "xT") if li == 0 else (self.hbuf, "hbuf")
        self.alloc_slots(True)
        gam, bgam = self.tile([128, c.KC], F32, "gamA")
        self.ld(gam, self.norm_mix[li], [bgam])
        TT = c.TTA
        n, bn = self.tile([128, c.KC, TT], BF16, "nA")
        nscr = self.norm_alloc(TT)
        save_top = self.top
        w = self.w_in[li]
        off = c.off
        ev = [0]

        def evac_copy(dst_ap, ps_ap, pb, dt, dname, func=None):
            s, bs = self.nstg()
            sv = s if dt == F32 else s.bitcast(BF16)
            shp = ps_ap.shape
            sv = sv[0:shp[0], 0:shp[1]]
            if func is not None:
                self.act(sv, ps_ap, func, [pb], [bs])
            elif ev[0] % 2 == 0:
                self.cp(sv, ps_ap, [pb], [bs], eng="dve")
            else:
                self.cp(sv, ps_ap, [pb], [bs], eng="act")
            ev[0] += 1
            self.st(dst_ap, sv, [bs], dname)

        for t0 in range(0, c.T, TT):
            self.top = save_top
            self.norm_tile(nscr, src, srcname, t0, TT, gam, bgam, out_bf=n, bout=bn)

            def fm(seg_off, ncols, dst, dname, dt, drow0, func=None):
                def ev_(col, mc, hh, ps, pb):
                    evac_copy(dst[drow0 + col:drow0 + col + mc, t0 + hh * 512:t0 + (hh + 1) * 512], ps[0:mc, :], pb,
                              dt, dname, func)
                self.lin_fm(lambda c0, cw: [(w[:, seg_off + c0:seg_off + c0 + cw], 0)], ncols,
                            lambda k, hh: n[:, k, hh * 512:(hh + 1) * 512], [bn], c.KC, TT, ev_)

            def tm(seg_off, ncols, dst, dname, dt):
                def ev_(c0, cw, tb, ps, pb):
                    evac_copy(dst[t0 + tb * 128:t0 + (tb + 1) * 128, c0:c0 + cw], ps[:, 0:cw], pb, dt, dname)
                self.lin_tm(lambda c0, cw: [(w[:, seg_off + c0:seg_off + c0 + cw], 0)], ncols,
                            lambda k, tb: n[:, k, tb * 128:(tb + 1) * 128], [bn], c.KC, TT, ev_)

            fm(off[0], 2 * c.WA, self.aqkT, "aqkT", F32, 0)
            tm(off[2], c.WA, self.av, "av", BF16)
            tm(off[3], c.WA, self.ao, "ao", F32)
            fm(off[4], 2 * c.HA, self.aifT, "aifT", F32, 0)
            fm(off[6], c.WB, self.bzuT, "bzuT", F32, 0)
            tm(off[6] + c.WB, c.WB, self.bzv, "bzv", F32)
            fm(off[7], 2 * c.WC, self.cqkT, "cqkT", BF16, 0)
            tm(off[9], c.WC, self.cv, "cv", BF16)
            fm(off[10], 3 * c.D, self.gates, "gates", F32, 0, func=AF.Sigmoid)

    def phase_b_gmlp(self, li):
        c = self.cfg
        self.phase()
        GB, WB = c.GB, c.WB
        wsf, bwsf = self.tile([128, GB, 128], F32, "wsf")
        self.ld(wsf, self.b_wsT[li].rearrange("g s t -> s g t"), [bwsf])
        wsb, bwsb = self.tile([128, GB, 128], BF16, "wsb")
        for g in range(GB):
            self.tt(wsb[:, g, :], wsf[:, g, :], self.cst[:, C_TRIU:C_TRIU + 128], ALU.mult, [bwsf, self.bcst], [bwsb])
        bsb, bbsb = self.tile([128, GB, 128], F32, "bsb")
        self.ld(bsb, self.b_bs[li].partition_broadcast(128).rearrange("p (g t) -> p g t", g=GB), [bbsb])
        lng, blng = self.tile([128, WB], F32, "lng")
        lnb, blnb = self.tile([128, WB], F32, "lnb")
        self.ld(lng, self.b_ln_g[li].partition_broadcast(128), [blng])
        self.ld(lnb, self.b_ln_b[li].partition_broadcast(128), [blnb])
        vin = [self.tile([128, WB], F32, "vin%d" % i) for i in range(2)]
        uin = [self.tile([128, GB, 128], F32, "uin%d" % i) for i in range(2)]
        t1, bt1 = self.tile([128, WB], F32, "g1")
        t2, bt2 = self.tile([128, WB], F32, "g2")
        vln, bvln = self.tile([128, WB], BF16, "vln")
        yb = [self.tile([128, GB, 128], BF16, "yb%d" % i) for i in range(2)]
        st_, bst = self.tile([128, 8], F32, "lnstat")
        ug, bug = self.tile([128, GB, 128], F32, "ug")

        def gelu(x, bx, outp, bo):
            self.tt(t1, x, x, ALU.mult, [bx], [bt1])
            self.ts(t1, t1, 0.044715, 1.0, ALU.mult, ALU.add, [bt1], [bt1])
            self.tt(t1, t1, x, ALU.mult, [bt1, bx], [bt1])
            self.act(t1, t1, AF.Sigmoid, [bt1], [bt1], scale=1.5957691216057308)
            self.tt(outp, t1, x, ALU.mult, [bt1, bx], [bo])

        for tb in range(c.NCH):
            v, bv = vin[tb % 2]
            u, bu = uin[tb % 2]
            y, by = yb[tb % 2]
            self.ld(v, self.bzv[tb * 128:(tb + 1) * 128, :], [bv], reads=[self.dbuf("bzv")])
            self.ld(u, self.bzuT.rearrange("(g d) t -> d g t", g=GB)[:, :, tb * 128:(tb + 1) * 128], [bu],
                    reads=[self.dbuf("bzuT")])
            gelu(v, bv, t2, bt2)
            self.act(t1, t2, AF.Copy, [bt2], [bt1, bst], accum_out=st_[:, 0:1])
            self.act(t1, t2, AF.Square, [bt2], [bt1, bst], accum_out=st_[:, 1:2])
            self.ts(st_[:, 2:3], st_[:, 0:1], 1.0 / WB, None, ALU.mult, None, [bst], [bst])
            self.tt(st_[:, 3:4], st_[:, 2:3], st_[:, 2:3], ALU.mult, [bst], [bst])
            self.stt(st_[:, 4:5], st_[:, 1:2], 1.0 / WB, st_[:, 3:4], ALU.mult, ALU.subtract, [bst], [bst])
            self.act(st_[:, 5:6], st_[:, 4:5], AF.Sqrt, [bst, self.bcc], [bst], bias=self.cc[:, 0:1])
            self.recip(st_[:, 6:7], st_[:, 5:6], [bst], [bst])
            self.ts(t2, t2, st_[:, 2:3], st_[:, 6:7], ALU.subtract, ALU.mult, [bt2, bst], [bt2])
            self.tt(t2, t2, lng, ALU.mult, [bt2, blng], [bt2])
            self.tt(vln, t2, lnb, ALU.add, [bt2, blnb], [bvln])
            ps0, pb0 = self.ps[0], self.psb[0]
            ps1, pb1 = self.ps[1], self.psb[1]
            for g in range(GB):
                ps, pb = (ps0, pb0) if g < 4 else (ps1, pb1)
                self.mm(ps[:, (g % 4) * 128:(g % 4 + 1) * 128], vln[:, g * 128:(g + 1) * 128], wsb[:, g, :], True, True,
                        [bvln, bwsb], [pb])
            gelu(u.rearrange("p g t -> p (g t)"), bu, ug.rearrange("p g t -> p (g t)"), bug)
            for half in range((GB + 3) // 4):
                ps, pb = (ps0, pb0) if half == 0 else (ps1, pb1)
                ng = min(4, GB - half * 4)
                sl = slice(half * 4, half * 4 + ng)
                tv = t2[:, half * 512:half * 512 + ng * 128].rearrange("p (g t) -> p g t", g=ng)
                self.tt(tv, ps[:, 0:ng * 128].rearrange("p (g t) -> p g t", g=ng), bsb[:, sl, :], ALU.add,
                        [pb, bbsb], [bt2])
                self.tt(y[:, sl, :], tv, ug[:, sl, :], ALU.mult, [bt2, bug], [by])
            self.st(self.yT[c.WA:c.WA + WB, :].rearrange("(g d) t -> d g t", g=GB)[:, :, tb * 128:(tb + 1) * 128],
                    y, [by], "yT")

    def phase_b_attn(self, li):
        c = self.cfg
        self.phase()
        HC, NCH, T = c.HC, c.NCH, c.T
        lam_init = 0.8 - 0.6 * math.exp(-0.3 * li)
        lt, blt = self.tile([1, 256], F32, "lam")
        self.ld(lt, self.c_lambda[li:li + 1, :], [blt])
        l2, bl2 = self.tile([1, 8], F32, "lam2")
        self.tt(lt[:, 0:64], lt[:, 0:64], lt[:, 64:128], ALU.mult, [blt], [blt])
        self.tt(lt[:, 128:192], lt[:, 128:192], lt[:, 192:256], ALU.mult, [blt], [blt])
        self.reduce(l2[:, 0:1], lt[:, 0:64], ALU.add, [blt], [bl2])
        self.reduce(l2[:, 1:2], lt[:, 128:192], ALU.add, [blt], [bl2])
        self.act(l2[:, 2:4], l2[:, 0:2], AF.Exp, [bl2], [bl2])
        self.tt(l2[:, 4:5], l2[:, 3:4], l2[:, 2:3], ALU.subtract, [bl2], [bl2])
        self.ts(l2[:, 5:6], l2[:, 4:5], -lam_init, None, ALU.add, None, [bl2], [bl2])
        ps, pb = self.nps()
        self.mm(ps[:, 0:1], self.cst[0:1, C_SEL:C_SEL + 128], l2[:, 5:6], True, True, [self.bcst, bl2], [pb])
        nlam, bnlam = self.tile([128, 1], F32, "nlam")
        self.cp(nlam, ps[:, 0:1], [pb], [bnlam])
        cn, bcn = self.tile([128, 128], F32, "cn")
        self.ld(cn, self.c_norm[li].partition_broadcast(128), [bcn])
        self.ts(cn, cn, 1.0 - lam_init, None, ALU.mult, None, [bcn], [bcn])
        qk = [(self.tile([128, T], BF16, "aq%d" % i), self.tile([128, T], BF16, "ak%d" % i),
               self.tile([128, NCH, 129], BF16, "avg%d" % i)) for i in range(2)]
        for i in range(2):
            self.memset(qk[i][2][0][:, :, 128:129], 1.0, [qk[i][2][1]])
        yTh = [self.tile([128, T], BF16, "ayT%d" % i) for i in range(2)]
        pT = [self.tile([128, 512], BF16, "pT%d" % i) for i in range(4)]
        tmp = [self.tile([128, 128], F32, "atmp%d" % i) for i in range(2)]
        fin, bfin = self.tile([128, 16], F32, "afin")
        of, bof = self.tile([128, 128], F32, "aof")
        of2, bof2 = self.tile([128, 128], F32, "aof2")
        ob, bob = self.tile([128, 128], BF16, "aob")
        p_rr = [0]
        for h in range(HC):
            (q, bq), (k, bk), (v, bv) = qk[h % 2]
            yt, byt = yTh[h % 2]
            self.ld(q, self.cqkT[h * 128:(h + 1) * 128, :], [bq], reads=[self.dbuf("cqkT")])
            self.ld(k, self.cqkT[c.WC + h * 128:c.WC + (h + 1) * 128, :], [bk], reads=[self.dbuf("cqkT")])
            self.ld(v[:, :, 0:128], self.cv[:, h * 128:(h + 1) * 128].rearrange("(c p) d -> p c d", p=128), [bv],
                    reads=[self.dbuf("cv")])
            c31 = self.c31[:, h:h + 1]
            for G in range((NCH + 3) // 4):
                qb0 = 4 * G
                nq = min(4, NCH - qb0)
                def acc(s, j):
                    b = 4 + s * 2 + j // 2
                    return self.ps[b][:, (j % 2) * 129:(j % 2) * 129 + 129], self.psb[b]
                for b_ in range(4, 8):
                    self.mm(self.ps[b_][:, 0:258], self.zb[0:1, 0:128], self.zb[0:1, 0:258], True, False,
                            [self.bzb], [self.psb[b_]])
                for kb in range(0, qb0 + nq):
                    far_all = kb <= qb0 - 2
                    if far_all:
                        groups = [(0, nq)]
                    else:
                        groups = [(j, 1) for j in range(nq) if qb0 + j >= kb]
                    for (j0, nj) in groups:
                        d = (qb0 + j0) - kb
                        for s in range(2):
                            ps, pb = self.nps(0, 4)
                            w = nj * 128
                            self.mm(ps[:, 0:w], k[s * 64:(s + 1) * 64, kb * 128:(kb + 1) * 128],
                                    q[s * 64:(s + 1) * 64, (qb0 + j0) * 128:(qb0 + j0) * 128 + w], True, True,
                                    [bk, bq], [pb])
                            p, bp = pT[p_rr[0] % 4]
                            p_rr[0] += 1
                            if d >= 2:
                                self.act(p[:, 0:w], ps[:, 0:w], AF.Exp, [pb, self.bc31], [bp], bias=c31, scale=0.125)
                            else:
                                bt, bbt = (self.BD, self.bBD) if d == 0 else (self.BO, self.bBO)
                                tt_, btt = tmp[s]
                                self.stt(tt_, ps[:, 0:128], 0.125, bt[:, h, :], ALU.mult, ALU.add, [pb, bbt], [btt])
                                self.act(p[:, 0:128], tt_, AF.Exp, [btt], [bp])
                            for jj in range(nj):
                                j = j0 + jj
                                o, bo = acc(s, j)
                                self.mm(o, p[:, jj * 128:(jj + 1) * 128], v[:, kb, :], False, kb == qb0 + j,
                                        [bp, bv], [bo])
                for j in range(nq):
                    o1, bo1 = acc(0, j)
                    o2, bo2 = acc(1, j)
                    self.recip(fin[:, 0:1], o1[:, 128:129], [bo1], [bfin])
                    self.recip(fin[:, 1:2], o2[:, 128:129], [bo2], [bfin])
                    self.tt(fin[:, 2:3], fin[:, 1:2], nlam, ALU.mult, [bfin, bnlam], [bfin])
                    self.ts(of, o1[:, 0:128], fin[:, 0:1], None, ALU.mult, None, [bo1, bfin], [bof])
                    self.stt(of, o2[:, 0:128], fin[:, 2:3], of, ALU.mult, ALU.add, [bo2, bfin, bof], [bof])
                    self.act(of2, of, AF.Square, [bof], [bof2, bfin], accum_out=fin[:, 3:4])
                    self.act(fin[:, 4:5], fin[:, 3:4], AF.Sqrt, [bfin, self.bcc], [bfin], bias=self.cc[:, 0:1],
                             scale=1.0 / 128)
                    self.recip(fin[:, 5:6], fin[:, 4:5], [bfin], [bfin])
                    self.stt(ob, of, fin[:, 5:6], cn, ALU.mult, ALU.mult, [bof, bfin, bcn], [bob])
                    ps, pb = self.nps(0, 4)
                    self.mm(ps[:, 0:128], ob, self.idb, True, True, [bob, self.bidb], [pb])
                    self.cp(yt[:, (qb0 + j) * 128:(qb0 + j + 1) * 128], ps[:, 0:128], [pb], [byt], eng="act")
            r0 = c.WA + c.WB + h * 128
            self.st(self.yT[r0:r0 + 128, :], yt, [byt], "yT")

    def phase_b_mlstm(self, li):
        c = self.cfg
        self.phase()
        HA, NCH, T = c.HA, c.NCH, c.T
        kscale = 128.0 ** -0.5
        self._nxb = 3
        Gm, bGm = self.tile([HA, T], F32, "Gm")
        Utm, bUtm = self.tile([128, NCH * HA], F32, "Utm")
        EXPN, bEXPN = self.tile([128, NCH * HA], F32, "EXPN")
        keep_top = self.top
        gi, bgi = self.tile([HA, T], F32, "gi")
        gf, bgf = self.tile([HA, T], F32, "gf")
        Bc, bBc = self.tile([HA, T], F32, "Bc")
        U, bU = self.tile([HA, T], F32, "U")
        one12, bone12 = self.tile([HA, T], F32, "one12")
        gb, bgb = self.tile([HA, 4], F32, "gb")
        self.memset(one12, 1.0, [bone12])
        self.ld(gi, self.aifT[0:HA, :], [bgi], reads=[self.dbuf("aifT")])
        self.ld(gf, self.aifT[HA:2 * HA, :], [bgf], reads=[self.dbuf("aifT")])
        self.ld(gb[:, 0:1], self.a_gate_bias[li, 0:HA, :], [bgb])
        self.ld(gb[:, 1:2], self.a_gate_bias[li, HA:2 * HA, :], [bgb])
        self.ts(gb[:, 2:3], gb[:, 1:2], -1.0, None, ALU.mult, None, [bgb], [bgb])
        self.act(gf, gf, AF.Exp, [bgf, bgb], [bgf], bias=gb[:, 2:3], scale=-1.0)
        self.act(gf, gf, AF.Ln, [bgf, self.bcc], [bgf], bias=self.cc[0:HA, 1:2])
        self.ts(gf, gf, -1.0, None, ALU.mult, None, [bgf], [bgf])
        self.scan(Bc, one12, gf, 0.0, ALU.mult, ALU.add, [bone12, bgf], [bBc])
        self.stt(U, gi, gb[:, 0:1], Bc, ALU.add, ALU.subtract, [bgi, bgb, bBc], [bU])
        self.scan(Gm, one12, U, 0.0, ALU.mult, ALU.max, [bone12, bU], [bGm])
        self.stt(gi, Bc, -1.0, Gm, ALU.mult, ALU.subtract, [bBc, bGm], [bgi])
        for (srcT, bsrc, dst, bdst, ex) in ((U, bU, Utm, bUtm, False), (gi, bgi, EXPN, bEXPN, True)):
            for c0 in range(0, NCH, 32):
                ncc = min(32, NCH - c0)
                ps, pb = self.nps()
                for cc_ in range(ncc):
                    ch = c0 + cc_
                    self.mm(ps[:, cc_ * HA:(cc_ + 1) * HA], srcT[:, ch * 128:(ch + 1) * 128],
                            self.cst[0:HA, C_ID:C_ID + HA], True, True, [bsrc, self.bcst], [pb])
                if ex:
                    self.act(dst[:, c0 * HA:(c0 + ncc) * HA], ps[:, 0:ncc * HA], AF.Exp, [pb], [bdst])
                else:
                    self.cp(dst[:, c0 * HA:(c0 + ncc) * HA], ps[:, 0:ncc * HA], [pb], [bdst])
        self.P.barrier()
        self.top = keep_top
        cw, bcw = self.tile([128, 2 * HA, 4], F32, "convw")
        self.ld(cw, self.a_conv[li].rearrange("(c p) j -> p c j", p=128), [bcw])
        an, ban = self.tile([128, c.WA], F32, "anorm")
        self.ld(an, self.a_norm[li].partition_broadcast(128), [ban])
        xin = [self.tile([128, T], F32, "mx%d" % i) for i in range(2)]
        accq, baccq = self.tile([128, T], F32, "macc")
        qT, bqT = self.tile([128, T], BF16, "mqT")
        kT, bkT = self.tile([128, T], BF16, "mkT")
        vaug, bvaug = self.tile([128, NCH, 129], BF16, "mvaug")
        self.memset(vaug[:, :, 128:129], 1.0, [bvaug])
        aot, baot = self.tile([128, NCH, 128], F32, "mao")
        yTh, byTh = self.tile([128, T], BF16, "myT")
        Gends, bGends = self.tile([128, NCH + 1], F32, "Gends")
        Dt = [self.tile([128, 128], F32, "mD%d" % i) for i in range(2)]
        Wt = [self.tile([128, 128], BF16, "mW%d" % i) for i in range(2)]
        Et = [self.tile([128, 128], F32, "mE%d" % i) for i in range(2)]
        qp = [self.tile([128, 128], BF16, "mqp%d" % i) for i in range(2)]
        kw = [self.tile([128, 128], BF16, "mkw%d" % i) for i in range(2)]
        Cf, bCf = self.tile([128, 129], F32, "mCf")
        Cb, bCb = self.tile([128, 129], BF16, "mCb")
        sm, bsm = self.tile([128, 16], F32, "msm")
        hg, bhg = self.tile([128, 128], F32, "mhg")
        hg2, bhg2 = self.tile([128, 128], F32, "mhg2")
        yb_, byb = self.tile([128, 128], BF16, "myb")

        def conv_silu(row0, cidx, outT, boutT, x, bx):
            self.ld(x, self.aqkT[row0:row0 + 128, :], [bx], reads=[self.dbuf("aqkT")])
            self.ts(accq, x, cw[:, cidx, 3:4], None, ALU.mult, None, [bx, bcw], [baccq])
            for sh in (1, 2, 3):
                self.stt(accq[:, sh:T], x[:, 0:T - sh], cw[:, cidx, 3 - sh:4 - sh], accq[:, sh:T], ALU.mult, ALU.add,
                         [bx, bcw, baccq], [baccq])
            self.act(outT, accq, AF.Silu, [baccq], [boutT])

        for h in range(HA):
            conv_silu(h * 128, h, qT, bqT, *xin[0])
            conv_silu(c.WA + h * 128, HA + h, kT, bkT, *xin[1])
            self.ld(vaug[:, :, 0:128], self.av[:, h * 128:(h + 1) * 128].rearrange("(c p) d -> p c d", p=128), [bvaug],
                    reads=[self.dbuf("av")])
            self.ld(aot, self.ao[:, h * 128:(h + 1) * 128].rearrange("(c p) d -> p c d", p=128), [baot],
                    reads=[self.dbuf("ao")])
            self.act(aot.rearrange("p c d -> p (c d)"), aot.rearrange("p c d -> p (c d)"), AF.Sigmoid, [baot], [baot])
            self.memset(Cf, 0.0, [bCf])
            self.memset(Cb, 0.0, [bCb])
            self.memset(Gends[:, 0:1], 0.0, [bGends])
            sel = self.cst[0:HA, C_SEL + h * 128:C_SEL + (h + 1) * 128]
            for g0 in range(0, NCH, 4):
                ng = min(4, NCH - g0)
                wcols = ng * 128
                psG, pbG = self.ps[6], self.psb[6]
                psM, pbM = self.ps[7], self.psb[7]
                self.mm(psG[:, 0:wcols], sel, Gm[:, g0 * 128:g0 * 128 + wcols], True, True, [self.bcst, bGm], [pbG])
                self.mm(psM[:, 0:wcols], sel, Gm[:, g0 * 128:g0 * 128 + wcols], True, False, [self.bcst, bGm], [pbM])
                self.mm(psM[:, 0:wcols], self.cst[:, C_ID:C_ID + 128], self.cst[:, C_BIG:C_BIG + wcols], False, True,
                        [self.bcst], [pbM])
                for cc_ in range(ng):
                    self.cp(Gends[:, g0 + cc_ + 1:g0 + cc_ + 2], psG[:, cc_ * 128 + 127:cc_ * 128 + 128], [pbG],
                            [bGends])
                for cc_ in range(ng):
                    ch = g0 + cc_
                    cs = slice(cc_ * 128, (cc_ + 1) * 128)
                    ts_ = slice(ch * 128, (ch + 1) * 128)
                    ucol = Utm[:, ch * HA + h:ch * HA + h + 1]
                    gprev = Gends[:, ch:ch + 1]
                    gend = Gends[:, ch + 1:ch + 2]
                    D_, bD = Dt[ch % 2]
                    W_, bW = Wt[ch % 2]
                    E_, bE = Et[ch % 2]
                    qp_, bqp = qp[ch % 2]
                    kw_, bkw = kw[ch % 2]
                    psS, pbS = self.nps(0, 2)
                    self.mm(psS[:, 0:128], kT[:, ts_], qT[:, ts_], True, True, [bkT, bqT], [pbS])
                    self.act(D_, psM[:, cs], AF.Exp, [pbM, bUtm], [bD], bias=ucol, scale=-1.0)
                    self.stt(W_, psS[:, 0:128], kscale, D_, ALU.mult, ALU.mult, [pbS, bD], [bW])
                    self.act(E_, psG[:, cs], AF.Exp, [pbG, bGends], [bE], bias=gprev, scale=-1.0)
                    self.tt(qp_, qT[:, ts_], E_, ALU.mult, [bqT, bE], [bqp])
                    psO, pbO = self.nps(2, 4)
                    self.mm(psO[:, 0:129], qp_, Cb, True, False, [bqp, bCb], [pbO])
                    self.mm(psO[:, 0:129], W_, vaug[:, ch, :], False, True, [bW, bvaug], [pbO])
                    self.act(sm[:, 0:1], gend, AF.Exp, [bGends, bUtm], [bsm], bias=ucol, scale=-1.0)
                    self.act(sm[:, 1:2], gend, AF.Exp, [bGends], [bsm], bias=gprev, scale=-1.0)
                    psK, pbK = self.nps(4, 6)
                    self.mm(psK[:, 0:128], kT[:, ts_], self.idb, True, True, [bkT, self.bidb], [pbK])
                    self.ts(kw_, psK[:, 0:128], sm[:, 0:1], kscale, ALU.mult, ALU.mult, [pbK, bsm], [bkw])
                    self.mm(psK[:, 256:385], kw_, vaug[:, ch, :], True, True, [bkw, bvaug], [pbK])
                    self.stt(Cf, Cf, sm[:, 1:2], psK[:, 256:385], ALU.mult, ALU.add, [bCf, bsm, pbK], [bCf])
                    self.cp(Cb, Cf, [bCf], [bCb], eng="act")
                    self.act(sm[:, 2:3], psO[:, 128:129], AF.Abs, [pbO], [bsm])
                    self.tt(sm[:, 3:4], sm[:, 2:3], EXPN[:, ch * HA + h:ch * HA + h + 1], ALU.max, [bsm, bEXPN], [bsm])
                    self.recip(sm[:, 4:5], sm[:, 3:4], [bsm], [bsm])
                    self.stt(hg, psO[:, 0:128], sm[:, 4:5], aot[:, ch, :], ALU.mult, ALU.mult, [pbO, bsm, baot], [bhg])
                    self.act(hg2, hg, AF.Square, [bhg], [bhg2, bsm], accum_out=sm[:, 5:6])
                    self.act(sm[:, 6:7], sm[:, 5:6], AF.Sqrt, [bsm, self.bcc], [bsm], bias=self.cc[:, 0:1],
                             scale=1.0 / 128)
                    self.recip(sm[:, 7:8], sm[:, 6:7], [bsm], [bsm])
                    self.stt(yb_, hg, sm[:, 7:8], an[:, h * 128:(h + 1) * 128], ALU.mult, ALU.mult, [bhg, bsm, ban],
                             [byb])
                    self.mm(psO[:, 256:384], yb_, self.idb, True, True, [byb, self.bidb], [pbO])
                    self.cp(yTh[:, ts_], psO[:, 256:384], [pbO], [byTh], eng="act")
            self.st(self.yT[h * 128:(h + 1) * 128, :], yTh, [byTh], "yT")

    def phase_c(self, li):
        c = self.cfg
        self.phase()
        KC, TT, D = c.KC, c.TT, c.D
        moe = (li % 2 == 1)
        hsrc, hsrcname = (self.xT, "xT") if li == 0 else (self.hbuf, "hbuf")
        self.alloc_slots(False)
        gam, bgam = self.tile([128, KC], F32, "gamC")
        self.ld(gam, self.norm_ffn[li], [bgam])
        if li == c.L - 1:
            gfin, bgfin = self.tile([128, KC], F32, "gamF")
            self.ld(gfin, self.final_norm, [bgfin])
        if moe:
            rsb, brs = self.tile([128, KC, c.NE], F32, "router")
            self.ld(rsb, self.router[0].rearrange("(k p) e -> p k e", p=128), [brs])
            cbc = [self.tile([128, 512], F32, "cbc%d" % e) for e in range(c.NE)]
            NFF = c.DFFE // 128
        else:
            NFF = c.DFF // 128
        NSPLIT = 1 if moe else 2
        NFH = (NFF + NSPLIT - 1) // NSPLIT
        R1, bR1 = self.tile([128, KC, TT], BF16, "R1")
        nR2 = max(KC, NFH)
        R2, bR2 = self.tile([128, nR2, TT], BF16, "R2")
        gt = [self.tile([128, 3, TT], F32, "gt%d" % i) for i in range(1)]
        self._nxb = 2
        hc = [self.tile([128, TT], F32, "hc%d" % i) for i in range(2)]
        m1, bm1 = self.tile([128, TT], F32, "m1")
        m2, bm2 = self.tile([128, TT], F32, "m2")
        nscr = self.norm_alloc(TT)
        rscr = self.route_alloc(TT) if moe else None
        save_top = self.top
        hrr = [0]
        grr = [0]

        def rmw(col, mc, ps, pb, src, srcname):
            x, bx = hc[hrr[0] % len(hc)]
            hrr[0] += 1
            self.ld(x[0:mc, :], src[col:col + mc, t0:t0 + TT], [bx], reads=[self.dbuf(srcname)])
            self.tt(x[0:mc, :], x[0:mc, :], ps[0:mc, :], ALU.add, [bx, pb], [bx])
            self.st(self.hbuf[col:col + mc, t0:t0 + TT], x[0:mc, :], [bx], "hbuf")

        for t0 in range(0, c.T, TT):
            self.top = save_top
            self.ld(R1, self.yT.rearrange("(k p) t -> p k t", p=128)[:, :, t0:t0 + TT], [bR1], reads=[self.dbuf("yT")])
            wa, wb, wc = self.w_br_a[li], self.w_br_b[li], self.w_br_c[li]
            c0 = 0
            blk = 384
            while c0 < D:
                cw = min(blk, D - c0)
                view, sb = self.wload([(wa[:, c0:c0 + cw], 0), (wb[:, c0:c0 + cw], c.HA), (wc[:, c0:c0 + cw], c.HA + c.GB)], cw)
                for j0 in range(0, cw, 128):
                    m = (c0 + j0) // 128
                    g, bg = gt[grr[0] % len(gt)]
                    grr[0] += 1
                    self.ld(g, self.gates.rearrange("(b d) t -> d b t", b=3)[m * 128:(m + 1) * 128, :, t0:t0 + TT], [bg],
                            reads=[self.dbuf("gates")])
                    pss = []
                    for br, (k0, nk) in enumerate(((0, c.HA), (c.HA, c.GB), (c.HA + c.GB, c.HC))):
                        ps, pb = self.nps()
                        for k in range(k0, k0 + nk):
                            self.mm(ps, view[:, k, j0:j0 + 128], R1[:, k, :], k == k0, k == k0 + nk - 1, [sb, bR1], [pb])
                        pss.append((ps, pb))
                    self.tt(m1, pss[0][0], g[:, 0, :], ALU.mult, [pss[0][1], bg], [bm1])
                    self.tt(m2, pss[1][0], g[:, 1, :], ALU.mult, [pss[1][1], bg], [bm2])
                    self.tt(m1, m1, m2, ALU.add, [bm1, bm2], [bm1], eng="pool")
                    self.tt(m2, pss[2][0], g[:, 2, :], ALU.mult, [pss[2][1], bg], [bm2])
                    self.tt(R2[:, m, :], m1, m2, ALU.add, [bm1, bm2], [bR2], eng="pool")
                c0 += cw
            wo = self.w_out[li]
            self.lin_fm(lambda c0, cw: [(wo[:, c0:c0 + cw], 0)], D, lambda k, hh: R2[:, k, :], [bR2], KC, TT,
                        lambda col, mc, hh, ps, pb: rmw(col, mc, ps, pb, hsrc, hsrcname))
            if moe:
                psl, pbl = self.ps[5], self.psb[5]
                self.norm_tile(nscr, self.hbuf, "hbuf", t0, TT, gam, bgam, out_bf=R1, bout=bR1,
                               router=(rsb, brs, psl, pbl))
                self.route(rscr, psl, pbl, cbc, TT)
            else:
                self.norm_tile(nscr, self.hbuf, "hbuf", t0, TT, gam, bgam, out_bf=R1, bout=bR1)
            if not moe:
                for sp_ in range(NSPLIT):
                    self.ffn(self.ffn_wg[0], self.ffn_wu[0], self.ffn_wd[0], sp_ * NFH, min(NFF, (sp_ + 1) * NFH),
                             R1, bR1, R2, bR2, m1, bm1, None, TT, rmw)
            else:
                for e in range(c.NE):
                    self.ffn(self.moe_wg[0, e], self.moe_wu[0, e], self.moe_wd[0, e], 0, NFF, R1, bR1, R2, bR2, m1, bm1,
                             cbc[e], TT, rmw)
            if li == c.L - 1:
                self.norm_tile(nscr, self.hbuf, "hbuf", t0, TT, gfin, bgfin, out_dram=self.outT)

    def ffn(self, wg, wu, wd, f_lo, f_hi, hn, bhn, a, ba, m1, bm1, cb, TT, rmw):
        c = self.cfg
        KC = c.KC
        f0 = f_lo * 128
        blk = 384
        ncols = f_hi * 128
        NFF = f_hi - f_lo
        while f0 < ncols:
            cw = min(blk, ncols - f0)
            vg, sg = self.wload([(wg[:, f0:f0 + cw], 0)], cw)
            vu, su = self.wload([(wu[:, f0:f0 + cw], 0)], cw)
            for j0 in range(0, cw, 128):
                f = (f0 + j0) // 128 - f_lo
                psg, pbg = self.nps()
                psu, pbu = self.nps()
                for k in range(KC):
                    self.mm(psg, vg[:, k, j0:j0 + 128], hn[:, k, :], k == 0, k == KC - 1, [sg, bhn], [pbg])
                for k in range(KC):
                    self.mm(psu, vu[:, k, j0:j0 + 128], hn[:, k, :], k == 0, k == KC - 1, [su, bhn], [pbu])
                self.act(m1, psg, AF.Silu, [pbg], [bm1])
                if cb is None:
                    self.tt(a[:, f, :], m1, psu, ALU.mult, [bm1, pbu], [ba])
                else:
                    self.tt(m1, m1, psu, ALU.mult, [bm1, pbu], [bm1])
                    self.tt(a[:, f, :], m1, cb[0], ALU.mult, [bm1, cb[1]], [ba], eng="pool")
            f0 += cw
        self.lin_fm(lambda c0, cw: [(wd[f_lo * 128:f_hi * 128, c0:c0 + cw], 0)], c.D, lambda k, hh: a[:, k, :], [ba], NFF, TT,
                    lambda col, mc, hh, ps, pb: rmw(col, mc, ps, pb, self.hbuf, "hbuf"))

    def route_alloc(self, TT):
        NE = self.cfg.NE
        ntb = TT // 128
        return (self.tile([128, ntb, NE], F32, "lg"), self.tile([128, ntb, NE], F32, "e1"),
                self.tile([128, ntb, NE], F32, "e2"), self.tile([128, ntb, NE], F32, "l2"),
                self.tile([128, ntb, 4], F32, "mx"), self.tile([NE, TT], F32, "cT"))

    def route(self, rscr, psl, pbl, cbc, TT):
        c = self.cfg
        NE = c.NE
        ntb = TT // 128
        (lg, blg), (e1, be1), (e2, be2), (l2, bl2), (mx, bmx), (cT, bcT) = rscr
        self.cp(lg.rearrange("p a b -> p (a b)"), psl[:, 0:ntb * NE], [pbl], [blg])
        for tb in range(ntb):
            self.reduce(mx[:, tb, 0:1], lg[:, tb, :], ALU.max, [blg], [bmx])
            self.ts(e1[:, tb, :], lg[:, tb, :], mx[:, tb, 0:1], None, ALU.is_equal, None, [blg, bmx], [be1])
            self.stt(l2[:, tb, :], e1[:, tb, :], -1e30, lg[:, tb, :], ALU.mult, ALU.add, [be1, blg], [bl2])
            self.reduce(mx[:, tb, 1:2], l2[:, tb, :], ALU.max, [bl2], [bmx])
            self.ts(e2[:, tb, :], l2[:, tb, :], mx[:, tb, 1:2], None, ALU.is_equal, None, [bl2, bmx], [be2])
            self.tt(mx[:, tb, 2:3], mx[:, tb, 0:1], mx[:, tb, 1:2], ALU.subtract, [bmx], [bmx])
            self.act(mx[:, tb, 2:3], mx[:, tb, 2:3], AF.Sigmoid, [bmx], [bmx])
            self.ts(mx[:, tb, 3:4], mx[:, tb, 2:3], -1.0, 1.0, ALU.mult, ALU.add, [bmx], [bmx])
            self.ts(e1[:, tb, :], e1[:, tb, :], mx[:, tb, 2:3], None, ALU.mult, None, [be1, bmx], [be1])
            self.stt(e1[:, tb, :], e2[:, tb, :], mx[:, tb, 3:4], e1[:, tb, :], ALU.mult, ALU.add, [be2, bmx, be1], [be1])
            ps, pb = self.nps(0, 5)
            self.mm(ps[0:NE, 0:128], e1[:, tb, :], self.cst[:, C_ID:C_ID + 128], True, True, [be1, self.bcst], [pb])
            self.cp(cT[:, tb * 128:(tb + 1) * 128], ps[0:NE, 0:128], [pb], [bcT])
        for e in range(NE):
            ps, pb = self.nps(0, 5)
            self.mm(ps[:, 0:TT], self.cst[0:NE, C_SEL + e * 128:C_SEL + (e + 1) * 128], cT, True, True,
                    [self.bcst, bcT], [pb])
            self.cp(cbc[e][0], ps[:, 0:TT], [pb], [cbc[e][1]], eng="act")

    def build(self, phases=None):
        c = self.cfg
        self.setup()
        for li in range(c.L):
            if phases is None or ("a%d" % li) in phases:
                self.phase_a(li)
            if phases is None or ("g%d" % li) in phases:
                self.phase_b_gmlp(li)
            if phases is None or ("t%d" % li) in phases:
                self.phase_b_attn(li)
            if phases is None or ("m%d" % li) in phases:
                self.phase_b_mlstm(li)
            if phases is None or ("c%d" % li) in phases:
                self.phase_c(li)
        self.P.barrier()
        last = self.P.epoch
        self.P.wait_all_at_end("sp", self.out_stores[-64:] + [last])
        self.P.emit()
        return self.nc


def host_layout(cfg, inputs, b):
    c = cfg
    f = lambda a: np.ascontiguousarray(np.asarray(a, dtype=np.float32))
    L = c.L
    m = {}
    m["xT"] = f(np.asarray(inputs["x"])[b].T)
    m["norm_mix"] = f(np.asarray(inputs["norm_mix"]).reshape(L, c.KC, 128).transpose(0, 2, 1))
    m["w_in"] = f(inputs["w_in"])
    m["a_gate_bias"] = f(np.asarray(inputs["a_gate_bias"]).reshape(L, 2 * c.HA, 1))
    m["a_conv"] = f(np.asarray(inputs["a_conv"]).transpose(0, 2, 1))
    m["a_norm"] = f(inputs["a_norm"])
    m["b_ln_g"] = f(inputs["b_ln_g"])
    m["b_ln_b"] = f(inputs["b_ln_b"])
    m["b_wsT"] = f(np.asarray(inputs["b_ws"]).transpose(0, 1, 3, 2))
    m["b_bs"] = f(np.asarray(inputs["b_bs"]).reshape(L, c.GB * 128))
    m["c_lambda"] = f(np.asarray(inputs["c_lambda"]).reshape(L, 256))
    m["c_norm"] = f(inputs["c_norm"])
    m["rel_bias"] = f(inputs["rel_bias"])
    for k in ("w_br_a", "w_br_b", "w_br_c", "w_out", "ffn_wg", "ffn_wu", "ffn_wd", "router", "moe_wg", "moe_wu",
              "moe_wd"):
        m[k] = f(inputs[k])
    m["norm_ffn"] = f(np.asarray(inputs["norm_ffn"]).reshape(L, c.KC, 128).transpose(0, 2, 1))
    m["final_norm"] = f(np.asarray(inputs["final_norm"]).reshape(c.KC, 128).T)
    m["consts"] = make_consts()
    return m


_CACHE = {}


def kernel(**inputs):
    x = np.asarray(inputs["x"])
    B, T, D = x.shape
    cfg = Cfg(D=D, T=T, NE=np.asarray(inputs["router"]).shape[-1])
    key = (D, T)
    if key not in _CACHE:
        _CACHE[key] = Builder(cfg).build()
    nc = _CACHE[key]
    in_maps = [host_layout(cfg, inputs, b) for b in range(B)]
    res = run_bass_kernel_spmd(nc, in_maps, core_ids=list(range(B)))
    out = np.stack([np.ascontiguousarray(res.results[b]["outT"].T) for b in range(B)], axis=0)
    return out.astype(np.float32)
```
